# Optimizing a Trainium2 kernel written in Bass

```python
import jax, jax.numpy as jnp
from jax import lax
import numpy as np

D_MODEL = 1024
BATCH = 8
SEQ = 4096
DEPTH = 2

CHUNK = 64
EXPAND = 2
D_INNER = EXPAND * D_MODEL
POOL_WINDOWS = (2, 4, 8, 16)
N_POOL_GROUPS = len(POOL_WINDOWS)
POOL_GROUP = D_INNER // N_POOL_GROUPS
HGRN_HEAD_DIM = 128
HGRN_HEADS = D_INNER // HGRN_HEAD_DIM
N_MIXERS = 2
N_POOL_LAYERS = (DEPTH + 1) // 2
N_HGRN_LAYERS = DEPTH // 2
EPS = 1e-6

kernel_name = "hybrid_pool_hgrn2_stream_encoder"


def rms_norm(x, g):
    xf = x.astype(jnp.float32)
    y = xf * lax.rsqrt(jnp.mean(xf * xf, axis=-1, keepdims=True) + EPS)
    return (y * g.astype(jnp.float32)).astype(x.dtype)


def multiscale_pool(v):
    T = v.shape[1]
    vf = v.astype(jnp.float32)
    cs = jnp.cumsum(vf, axis=1)
    pos = jnp.arange(T, dtype=jnp.float32)
    outs = []
    for g, w in enumerate(POOL_WINDOWS):
        lo, hi = g * POOL_GROUP, (g + 1) * POOL_GROUP
        c = cs[..., lo:hi]
        prev = jnp.pad(c[:, :-w], ((0, 0), (w, 0), (0, 0)))
        cnt = jnp.minimum(pos + 1.0, float(w))[None, :, None]
        outs.append((c - prev) / cnt - vf[..., lo:hi])
    return jnp.stack(outs, axis=2).astype(v.dtype)


def pool_mixer(h, w_in, w_grp, scale, w_out):
    B, T, _ = h.shape
    u = h @ w_in
    v, gate = jnp.split(u, 2, axis=-1)
    p = multiscale_pool(v)
    m = jnp.einsum('btgc,gcd->btgd', p, w_grp).reshape(B, T, D_INNER) * scale
    return (m * jax.nn.silu(gate)) @ w_out


def hgrn2_chunk_scan(q, k, v, b):
    _, Bn, H, C, dk = q.shape
    dv = v.shape[-1]
    causal = jnp.tril(jnp.ones((C, C), dtype=bool))

    def step(S, xs):
        qc, kc, vc, bc = xs
        diff = bc[:, :, :, None, :] - bc[:, :, None, :, :]
        diff = jnp.where(causal[None, None, :, :, None], diff, -jnp.inf)
        A = jnp.einsum('bhtd,bhsd,bhtsd->bhts', qc, kc, jnp.exp(diff))
        o = (jnp.einsum('bhts,bhsv->bhtv', A, vc)
             + jnp.einsum('bhtd,bhdv->bhtv', qc * jnp.exp(bc), S))
        b_last = bc[:, :, -1:, :]
        S = (jnp.exp(b_last[:, :, 0, :])[..., None] * S
             + jnp.einsum('bhsd,bhsv->bhdv', kc * jnp.exp(b_last - bc), vc))
        return S, o

    S0 = jnp.zeros((Bn, H, dk, dv), jnp.float32)
    _, o = lax.scan(step, S0, (q, k, v, b))
    return o


def hgrn2_mixer(h, w_in, lb, norm_g, w_out):
    B, T, _ = h.shape
    N = T // CHUNK
    u = h @ w_in
    q, fz, i, gate = jnp.split(u, 4, axis=-1)
    fz = fz.astype(jnp.float32)
    lbf = lb.astype(jnp.float32)
    log_f = jnp.logaddexp(jnp.log(lbf), jnp.log1p(-lbf) + jax.nn.log_sigmoid(fz))
    k = (1.0 - lbf) * jax.nn.sigmoid(-fz)

    def to_chunks(a):
        return a.astype(jnp.float32).reshape(B, N, CHUNK, HGRN_HEADS, HGRN_HEAD_DIM).transpose(1, 0, 3, 2, 4)

    qc, kc, vc, lfc = to_chunks(q), to_chunks(k), to_chunks(i), to_chunks(log_f)
    bc = jnp.cumsum(lfc, axis=3)
    o = hgrn2_chunk_scan(qc, kc, vc, bc)
    o = o.transpose(1, 0, 3, 2, 4).reshape(B, T, HGRN_HEADS, HGRN_HEAD_DIM)
    o = o * lax.rsqrt(jnp.mean(o * o, axis=-1, keepdims=True) + EPS)
    o = (o * norm_g.astype(jnp.float32).reshape(HGRN_HEADS, HGRN_HEAD_DIM)).reshape(B, T, D_INNER)
    o = o.astype(h.dtype)
    return (o * jax.nn.silu(gate)) @ w_out


def setup_inputs(seed: int = 0) -> dict:
    key = jax.random.key(seed)
    ks = jax.random.split(key, 12)
    f32 = jnp.float32
    x = jax.random.normal(ks[0], (BATCH, SEQ, D_MODEL), f32)
    norm_g = 1.0 + 0.05 * jax.random.normal(ks[1], (DEPTH, D_MODEL), f32)
    pool_w_in = jax.random.normal(ks[2], (N_POOL_LAYERS, D_MODEL, 2 * D_INNER), f32) * D_MODEL ** -0.5
    pool_w_grp = jax.random.normal(ks[3], (N_POOL_LAYERS, N_POOL_GROUPS, POOL_GROUP, POOL_GROUP), f32) * POOL_GROUP ** -0.5
    pool_scale = 1.0 + 0.1 * jax.random.normal(ks[4], (N_POOL_LAYERS, D_INNER), f32)
    pool_w_out = jax.random.normal(ks[5], (N_POOL_LAYERS, D_INNER, D_MODEL), f32) * D_INNER ** -0.5
    hgrn_w_in = jax.random.normal(ks[6], (N_HGRN_LAYERS, D_MODEL, 4 * D_INNER), f32) * D_MODEL ** -0.5
    hgrn_lower_bounds = 0.5 * jax.random.normal(ks[7], (DEPTH, D_INNER), f32)
    hgrn_norm_g = 1.0 + 0.05 * jax.random.normal(ks[8], (N_HGRN_LAYERS, D_INNER), f32)
    hgrn_w_out = jax.random.normal(ks[9], (N_HGRN_LAYERS, D_INNER, D_MODEL), f32) * D_INNER ** -0.5
    final_g = 1.0 + 0.05 * jax.random.normal(ks[10], (D_MODEL,), f32)
    return {"x": x, "norm_g": norm_g, "pool_w_in": pool_w_in, "pool_w_grp": pool_w_grp,
            "pool_scale": pool_scale, "pool_w_out": pool_w_out, "hgrn_w_in": hgrn_w_in,
            "hgrn_lower_bounds": hgrn_lower_bounds, "hgrn_norm_g": hgrn_norm_g,
            "hgrn_w_out": hgrn_w_out, "final_g": final_g}


def reference(x, norm_g, pool_w_in, pool_w_grp, pool_scale, pool_w_out, hgrn_w_in,
              hgrn_lower_bounds, hgrn_norm_g, hgrn_w_out, final_g):
    p = jax.nn.softmax(hgrn_lower_bounds.astype(jnp.float32), axis=0)
    lbs = jnp.cumsum(p, axis=0) - p[0:1]
    h = x
    for layer in range(DEPTH):
        hn = rms_norm(h, norm_g[layer])
        j = layer // N_MIXERS
        if layer % N_MIXERS == 0:
            y = pool_mixer(hn, pool_w_in[j], pool_w_grp[j], pool_scale[j], pool_w_out[j])
        else:
            y = hgrn2_mixer(hn, hgrn_w_in[j], lbs[layer], hgrn_norm_g[j], hgrn_w_out[j])
        h = h + y.astype(h.dtype)
    return rms_norm(h, final_g)
```

```python
import numpy as np
from contextlib import ExitStack
import concourse.bass as bass
import concourse.mybir as mybir
from concourse.bass_utils import run_bass_kernel_spmd

F32 = mybir.dt.float32
BF16 = mybir.dt.bfloat16
ALU = mybir.AluOpType
AF = mybir.ActivationFunctionType

D_MODEL = 1024
SEQ = 4096
D_INNER = 2048
T = 512
EPS = 1e-6
NSLOT = 6
WINDOWS = (2, 4, 8, 16)
HEAD_STAGGER = 4
PROJ_PACE = 8.0
NPG = 4


class Buf:
    __slots__ = ("name", "writes", "reads", "dsem", "dcnt", "tw", "tr")

    def __init__(self, name=""):
        self.name = name
        self.writes = {}
        self.reads = {}
        self.dsem = None
        self.dcnt = 0
        self.tw = 0.0
        self.tr = 0.0


class Ctx:
    ENG = ("pe", "act", "dve", "pool", "sp")

    def __init__(self, nc, es):
        self.nc = nc
        self.es = es
        self.e = {"pe": nc.tensor, "act": nc.scalar, "dve": nc.vector, "pool": nc.gpsimd, "sp": nc.sync}
        self.sem = {k: es.enter_context(nc.semaphore("sem_" + k)) for k in self.ENG}
        self.cnt = {k: 0 for k in self.ENG}
        self.seen = {k: {} for k in self.ENG}
        self.nwait = 0
        self.ndsem = 0
        self.t_eng = {k: 0.0 for k in self.ENG}
        self.step_end = None

    COST = {"pe": (0.03, 0.00045), "act": (0.12, 0.00095), "dve": (0.10, 0.0011), "pool": (0.15, 0.0012), "sp": (0.1, 0.0)}

    def _time(self, eng, reads, writes, dur, occupy=None):
        t = self.t_eng[eng]
        for b in reads:
            if b.tw > t:
                t = b.tw
        for b in writes:
            if b.tw > t:
                t = b.tw
            if b.tr > t:
                t = b.tr
        end = t + dur
        self.t_eng[eng] = t + (dur if occupy is None else occupy)
        for b in reads:
            if end > b.tr:
                b.tr = end
        for b in writes:
            b.tw = end
        if self.step_end is None or end > self.step_end:
            self.step_end = end

    def _deps(self, eng, reads, writes, same_ok=False):
        need = {}
        for b in reads:
            for k, sv in b.writes.items():
                if k not in need or need[k][1] < sv[1]:
                    need[k] = sv
        for b in writes:
            for d in (b.writes, b.reads):
                for k, sv in d.items():
                    if k not in need or need[k][1] < sv[1]:
                        need[k] = sv
        seen = self.seen[eng]
        for k, (s, v) in need.items():
            if k == eng and same_ok:
                continue
            if seen.get(k, 0) >= v:
                continue
            self.e[eng].wait_ge(s, v)
            self.nwait += 1
            seen[k] = v

    def op(self, eng, fn, reads=(), writes=(), same_ok=False, acc=False, n=512):
        self._deps(eng, reads, writes, same_ok or acc)
        a_, b_ = self.COST[eng]
        self._time(eng, reads, writes, a_ + b_ * n)
        inst = fn(self.e[eng])
        self.cnt[eng] += 1
        tok = (self.sem[eng], self.cnt[eng])
        inst.then_inc(self.sem[eng], 1)
        for b in reads:
            b.reads[eng] = tok
        for b in writes:
            if acc:
                b.writes[eng] = tok
            else:
                b.writes = {eng: tok}
                b.reads = {}
        return inst

    def dma(self, q, out_ap, in_ap, reads=(), writes=(), us=6.0, **kw):
        self._deps(q, reads, writes)
        owner = writes[0] if writes else reads[0]
        kind = "sw" if q == "pool" else "hw"
        if owner.dsem is None:
            owner.dsem = {}
            owner.dcnt = {}
        if kind not in owner.dsem:
            owner.dsem[kind] = self.es.enter_context(self.nc.semaphore("dsem_%d" % self.ndsem))
            owner.dcnt[kind] = 0
            self.ndsem += 1
        owner.dcnt[kind] += 16
        inst = self.e[q].dma_start(out=out_ap, in_=in_ap, **kw)
        inst.then_inc(owner.dsem[kind], 16)
        key = ("dma", id(owner), kind)
        tok = (owner.dsem[kind], owner.dcnt[kind])
        for b in reads:
            b.reads[key] = tok
        for b in writes:
            b.writes = {key: tok}
            b.reads = {}
        return inst

    def wait_all(self, eng, bufs):
        for b in bufs:
            for d in (b.writes, b.reads):
                for k, (s, v) in d.items():
                    if self.seen[eng].get(k, 0) < v:
                        self.e[eng].wait_ge(s, v)
                        self.seen[eng][k] = v


def run_tasks(c, tasks, k):
    it = iter(tasks)
    active = []
    exhausted = False
    while True:
        while not exhausted and len(active) < k:
            nxt = next(it, None)
            if nxt is None:
                exhausted = True
                break
            t0 = min([a[1] for a in active], default=0.0)
            active.append([nxt[0], t0, nxt[1]])
        if not active:
            break
        a = min(active, key=lambda x: x[1])
        c.step_end = None
        try:
            next(a[0])
        except StopIteration:
            active.remove(a)
            continue
        if c.step_end is None:
            others = [x[1] for x in active if x is not a]
            a[1] = (min(others) if others else a[1]) + 0.05
        else:
            a[1] = c.step_end + a[2]


def _band_mats():
    out = np.zeros((128, 12, 128), np.float32)
    s = np.arange(128)[:, None]
    t = np.arange(128)[None, :]
    for g, w in enumerate(WINDOWS):
        d = t - s
        inwin = (d >= 0) & (d < w)
        eye = (d == 0).astype(np.float32)
        out[:, 3 * g + 0, :] = inwin.astype(np.float32) / w - eye
        d2 = t + 128 - s
        out[:, 3 * g + 1, :] = ((d2 >= 0) & (d2 < w)).astype(np.float32) / w
        cnt = np.minimum(t + 1, w).astype(np.float32)
        out[:, 3 * g + 2, :] = inwin.astype(np.float32) / cnt - eye
    return out.reshape(128, 12 * 128)


def _pcol(v):
    v = np.asarray(v, np.float32).reshape(-1, 128)
    return np.ascontiguousarray(v.T)


VC_G0, VC_G1, VC_FG, VC_PS, VC_L0, VC_L1, VC_HG = 0, 8, 16, 24, 40, 56, 72
NVC = 88


def build_nc(nt=SEQ // T, order=None):
    record = order is None
    seq = nt * T
    nc = bass.Bass("TRN2", target_bir_lowering=False)
    xT = nc.dram_tensor("xT", [D_MODEL, seq], F32, kind="ExternalInput").ap()
    outT = nc.dram_tensor("outT", [D_MODEL, seq], F32, kind="ExternalOutput").ap()
    pw_in = nc.dram_tensor("pw_in", [1024, 4096], F32, kind="ExternalInput").ap()
    pw_grp = nc.dram_tensor("pw_grp", [2048, 512], F32, kind="ExternalInput").ap()
    pw_out = nc.dram_tensor("pw_out", [2048, 1024], F32, kind="ExternalInput").ap()
    hw_in = nc.dram_tensor("hw_in", [1024, 8192], F32, kind="ExternalInput").ap()
    hw_out = nc.dram_tensor("hw_out", [2048, 1024], F32, kind="ExternalInput").ap()
    vecs = nc.dram_tensor("vecs", [128, NVC], F32, kind="ExternalInput").ap()
    bands = nc.dram_tensor("bands", [128, 12 * 128], F32, kind="ExternalInput").ap()
    cmisc = nc.dram_tensor("cmisc", [128, 256], F32, kind="ExternalInput").ap()
    NS = 36
    wscr = nc.dram_tensor("wscr", [NS, 128, 4096], BF16, kind="Internal").ap()

    xv = xT.rearrange("(c p) t -> p c t", p=128)
    ov = outT.rearrange("(c p) t -> p c t", p=128)

    with ExitStack() as es:
        c = Ctx(nc, es)
        sb = lambda n, shp, dt=F32: es.enter_context(nc.sbuf_tensor(n, shp, dt))

        hbuf = [sb("hbuf%d" % i, [128, 8, T]) for i in range(2)]
        hB = [[Buf("h%d_%d" % (i, k)) for k in range(8)] for i in range(2)]
        hn = sb("hn", [128, 8, T], BF16)
        hnB = [Buf("hn%d" % k) for k in range(8)]
        sq = [sb("sq%d" % i, [128, T], BF16) for i in range(2)]
        sqB = [Buf("sq%d" % i) for i in range(2)]
        lnt = sb("lnt", [128, T]); lntB = Buf("lnt")
        rstd, rstdB = lnt, lntB
        vtok = [sb("vtok%d" % i, [128, D_INNER], BF16) for i in range(5)]
        vtokB = [[Buf("vtok%d_%d" % (i, g)) for g in range(4)] for i in range(5)]
        pT = sb("pT", [128, 16, T], BF16)
        pTB = [Buf("pT%d" % k) for k in range(16)]
        vtok2 = pT[:].rearrange("p (b g) t -> p b (g t)", b=4)
        sg = sb("sg", [128, 16, T], BF16)
        sgB = [Buf("sg%d" % k) for k in range(16)]
        NA, NH, NB = 3, 5, 2
        R_t = [sb("R_t%d" % i, [128, T]) for i in range(NA)]; R_B = [Buf() for _ in range(NA)]
        L_t = [sb("L_t%d" % i, [128, T]) for i in range(NA)]; L_B = [Buf() for _ in range(NA)]
        B_t = [sb("B_t%d" % i, [128, T]) for i in range(NA)]; B_B = [Buf() for _ in range(NA)]
        Eq_t = [sb("Eq_t%d" % i, [128, T]) for i in range(NA)]; Eq_B = [Buf() for _ in range(NA)]
        kT_t = [sb("kT_t%d" % i, [128, T], BF16) for i in range(NH)]; kT_B = [Buf() for _ in range(NH)]
        qT_t = [sb("qT_t%d" % i, [128, T], BF16) for i in range(NH)]; qT_B = [Buf() for _ in range(NH)]
        ktok_t = [sb("ktok_t%d" % i, [128, T], BF16) for i in range(NH)]; ktok_B = [Buf() for _ in range(NH)]
        At_t = [sb("At_t%d" % i, [128, 128], BF16) for i in range(2 * NH)]; At_B = [Buf() for _ in range(2 * NH)]
        Sp_t = [sb("Sp_t%d" % i, [128, 128], BF16) for i in range(2 * NH)]; Sp_B = [Buf() for _ in range(2 * NH)]
        t1_t = [sb("t1_t%d" % i, [128, T]) for i in range(1)]; t1_B = [Buf() for _ in range(1)]
        rs_t = [sb("rs_t%d" % i, [128, T]) for i in range(1)]; rs_B = [Buf() for _ in range(1)]
        lc_t = [sb("lc_t%d" % i, [128, 4]) for i in range(NH)]; lc_B = [Buf() for _ in range(NH)]
        cv_t = [sb("cv_t%d" % i, [128, 4]) for i in range(NH)]; cv_B = [Buf() for _ in range(NH)]
        P_all = sb("P_all", [128, 16, 128]); P_B = [Buf("P%d" % k) for k in range(16)]
        carry = sb("carry", [128, 16]); carry_B = [Buf("carry%d" % k) for k in range(16)]
        vt = sb("vt", [128, NVC]); vtB = Buf("vt")
        lbt = sb("lbt", [128, 80]); lbB = Buf("lbt")
        bandb = sb("bandb", [128, 12 * 128], BF16); bandB = Buf("band")
        identb = sb("identb", [128, 128], BF16); identB = Buf("ident")
        maskf = sb("maskf", [128, 128]); maskB = Buf("mask")
        onesb = sb("onesb", [128, 128], BF16); onesbB = Buf("onesb")
        onesf = sb("onesf", [128, T]); onesfB = Buf("onesf")
        ring = [sb("ring%d" % i, [128, 8, 512], BF16) for i in range(NSLOT)]
        ringB = [Buf("ring%d" % i) for i in range(NSLOT)]
        scrB = [Buf("scr%d" % i) for i in range(NS)]

        psg = [es.enter_context(nc.psum_tensor("psg%d" % i, [128, 512], F32)) for i in range(NPG)]
        psgB = [Buf("psg%d" % i) for i in range(NPG)]
        pso = [es.enter_context(nc.psum_tensor("pso%d" % i, [128, 512], F32)) for i in range(NB)]
        psoB = [Buf("pso%d" % i) for i in range(NB)]
        psm = [es.enter_context(nc.psum_tensor("psm%d" % i, [128, 512], F32)) for i in range(NB)]
        psmB = [Buf("psm%d" % i) for i in range(NB)]
        st = {"pg": 0, "ev": 0}

        def pb():
            i = st["pg"] % NPG
            st["pg"] += 1
            return psg[i], psgB[i]

        def mm(ps_ap, psB_, lhsT, rhs, rd, first, last):
            c.op("pe", lambda e: e.matmul(ps_ap, lhsT=lhsT, rhs=rhs, start=first, stop=last),
                 reads=rd, writes=[psB_], same_ok=True, acc=not first, n=int(rhs.shape[-1]))

        def evac_copy(out_ap, ps_ap, psB_, outB):
            st["ev"] += 1
            if st["ev"] % 2:
                c.op("act", lambda e: e.activation(out=out_ap, in_=ps_ap, func=AF.Copy), reads=[psB_], writes=[outB])
            else:
                c.op("dve", lambda e: e.tensor_copy(out=out_ap, in_=ps_ap), reads=[psB_], writes=[outB])

        slot_src = []
        skey = {}

        def add_slot(key, src, nk):
            skey[key] = len(slot_src)
            slot_src.append((src, nk))

        pin = pw_in.rearrange("(kc p) f -> p kc f", p=128)
        for g in range(8):
            add_slot("pin%d" % g, pin[:, :, g * 512:(g + 1) * 512], 8)
        pgr = pw_grp.rearrange("(r p) f -> p r f", p=128)
        for g in range(4):
            add_slot("pgr%d" % g, pgr[:, g * 4:(g + 1) * 4, :], 4)
        pou = pw_out.rearrange("(kc p) f -> p kc f", p=128)
        for cg in range(2):
            for half in range(2):
                add_slot("pou%d%d" % (cg, half), pou[:, half * 8:(half + 1) * 8, cg * 512:(cg + 1) * 512], 8)
        hin = hw_in.rearrange("(kc p) f -> p kc f", p=128)
        for g in range(16):
            add_slot("hin%d" % g, hin[:, :, g * 512:(g + 1) * 512], 8)
        hou = hw_out.rearrange("(kc p) f -> p kc f", p=128)
        for cg in range(2):
            for half in range(2):
                add_slot("hou%d%d" % (cg, half), hou[:, half * 8:(half + 1) * 8, cg * 512:(cg + 1) * 512], 8)
        assert len(slot_src) == NS

        c.op("pool", lambda e: e.memset(onesf[:], 1.0), writes=[onesfB])
        c.op("pool", lambda e: e.memset(onesb[:], 1.0), writes=[onesbB])
        c.op("pool", lambda e: e.memset(P_all[:], 0.0), writes=P_B)
        c.op("pool", lambda e: e.memset(carry[:], 0.0), writes=carry_B)
        for i in range(2 * NH):
            c.op("pool", lambda e: e.memset(At_t[i][:], 0.0), writes=[At_B[i]])
        c.dma("sp", vt[:], vecs, writes=[vtB])
        c.dma("sp", maskf[:], cmisc[:, 128:256], writes=[maskB])
        c.dma("pool", identb[:], cmisc[:, 0:128], writes=[identB])
        c.dma("pool", bandb[:], bands, writes=[bandB])

        rs_ = {"emitted": 0, "acq": 0}
        free_slots = list(range(NSLOT))
        load_slot = {}
        order_rec = []
        total_loads = nt * NS

        def emit_load(n, key):
            s_ = skey[key]
            i = free_slots.pop(0)
            load_slot[n] = i
            src, nk = slot_src[s_]
            scr_v = wscr[s_].rearrange("p (kc f) -> p kc f", f=512)[:, 0:nk, :]
            if n < NS:
                c.dma("pool", ring[i][:, 0:nk, :], src, writes=[ringB[i]])
                c.dma("sp", scr_v, ring[i][:, 0:nk, :], reads=[ringB[i]], writes=[scrB[s_]])
            else:
                c.dma("sp", ring[i][:, 0:nk, :], scr_v, reads=[scrB[s_]], writes=[ringB[i]])
            rs_["emitted"] += 1

        def ring_pump():
            if record:
                return
            while rs_["emitted"] < total_loads and free_slots:
                if rs_["acq"] < 3 and rs_["emitted"] - rs_["acq"] >= 2:
                    break
                m = rs_["emitted"]
                emit_load(m, order[m])

        def acquire(key):
            n = rs_["acq"]
            rs_["acq"] += 1
            if record:
                order_rec.append(key)
                assert free_slots, "too many weight slots held at once"
                emit_load(n, key)
            else:
                assert order[n] == key, (n, key, order[n])
                assert n < rs_["emitted"], "ring underflow"
            i = load_slot[n]
            ring_pump()
            return ring[i], ringB[i], n

        def release(*ns):
            for n in ns:
                free_slots.append(load_slot.pop(n))
            ring_pump()

        c.op("dve", lambda e: e.tensor_tensor(out=lbt[:, 0:16], in0=vt[:, VC_L1:VC_L1 + 16], in1=vt[:, VC_L0:VC_L0 + 16], op=ALU.subtract),
             reads=[vtB], writes=[lbB])
        c.op("act", lambda e: e.activation(out=lbt[:, 0:16], in_=lbt[:, 0:16], func=AF.Exp), reads=[lbB], writes=[lbB])
        c.op("dve", lambda e: e.tensor_scalar(out=lbt[:, 0:16], in0=lbt[:, 0:16], scalar1=1.0, scalar2=None, op0=ALU.add), reads=[lbB], writes=[lbB])
        c.op("dve", lambda e: e.reciprocal(out=lbt[:, 16:32], in_=lbt[:, 0:16]), reads=[lbB], writes=[lbB])
        c.op("dve", lambda e: e.tensor_scalar(out=lbt[:, 32:48], in0=lbt[:, 16:32], scalar1=-1.0, scalar2=None, op0=ALU.mult), reads=[lbB], writes=[lbB])
        c.op("dve", lambda e: e.tensor_scalar(out=lbt[:, 48:64], in0=lbt[:, 32:48], scalar1=1.0, scalar2=None, op0=ALU.add), reads=[lbB], writes=[lbB])
        c.op("act", lambda e: e.activation(out=lbt[:, 64:80], in_=lbt[:, 16:32], func=AF.Ln), reads=[lbB], writes=[lbB])
        omlb = lambda hd: lbt[:, 16 + hd:17 + hd]
        lbc = lambda hd: lbt[:, 48 + hd:49 + hd]
        lnomlb = lambda hd: lbt[:, 64 + hd:65 + hd]
        nomlb = lambda hd: lbt[:, 32 + hd:33 + hd]

        ring_pump()

        def load_x(ti):
            i = ti % 2
            c.dma("sp", hbuf[i][:], xv[:, :, ti * T:(ti + 1) * T], writes=hB[i])

        def rms(h, hBl, inv_n_chunks, gcol, out_fn):
            ps, psB_ = pb()
            for k in range(8):
                j = k % 2
                c.op("act", lambda e: e.activation(out=sq[j][:], in_=h[:, k, :], func=AF.Square), reads=[hBl[k]], writes=[sqB[j]])
                mm(ps[:], psB_, onesb[:], sq[j][:], [onesbB, sqB[j]], k == 0, k == 7)
            c.op("act", lambda e: e.activation(out=lnt[:], in_=ps[:], func=AF.Ln, scale=1.0 / D_MODEL, bias=EPS), reads=[psB_], writes=[lntB])
            c.op("act", lambda e: e.activation(out=rstd[:], in_=lnt[:], func=AF.Exp, scale=-0.5), reads=[lntB], writes=[rstdB])

        def norm_to_hn(h, hBl, gc):
            rms(h, hBl, None, None, None)
            for k in range(8):
                c.op("dve", lambda e: e.scalar_tensor_tensor(out=hn[:, k, :], in0=h[:, k, :], scalar=vt[:, gc + k:gc + k + 1], in1=rstd[:],
                                                             op0=ALU.mult, op1=ALU.mult),
                     reads=[hBl[k], vtB, rstdB], writes=[hnB[k]])

        def out_proj(h, hBl, lay):
            for cg in range(2):
                w0, w0B, n0 = acquire("%sou%d0" % (lay, cg))
                w1, w1B, n1 = acquire("%sou%d1" % (lay, cg))
                for j in range(4):
                    oc = cg * 4 + j
                    ps, psB_ = pb()
                    for half, (w, wB) in enumerate(((w0, w0B), (w1, w1B))):
                        for kc in range(8):
                            k = half * 8 + kc
                            mm(ps[:], psB_, w[:, kc, j * 128:(j + 1) * 128], sg[:, k, :], [wB, sgB[k]], k == 0, k == 15)
                    c.op("dve", lambda e: e.tensor_tensor(out=h[:, oc, :], in0=ps[:], in1=h[:, oc, :], op=ALU.add),
                         reads=[psB_, hBl[oc]], writes=[hBl[oc]])
                release(n0, n1)

        load_x(0)
        for ti in range(nt):
            hi = ti % 2
            h, hBl = hbuf[hi], hB[hi]
            if ti + 1 < nt:
                load_x(ti + 1)
            norm_to_hn(h, hBl, VC_G0)
            for g in range(4):
                w, wB, wn = acquire("pin%d" % g)
                for tb in range(4):
                    blk = (ti * 4 + tb) % 5
                    ps, psB_ = pb()
                    for kc in range(8):
                        mm(ps[:], psB_, hn[:, kc, tb * 128:(tb + 1) * 128], w[:, kc, :], [hnB[kc], wB], kc == 0, kc == 7)
                    evac_copy(vtok[blk][:, g * 512:(g + 1) * 512], ps[:], psB_, vtokB[blk][g])
                release(wn)
            for g in range(4):
                w, wB, wn = acquire("pin%d" % (4 + g))
                for j in range(4):
                    dc = g * 4 + j
                    ps, psB_ = pb()
                    for kc in range(8):
                        mm(ps[:], psB_, w[:, kc, j * 128:(j + 1) * 128], hn[:, kc, :], [wB, hnB[kc]], kc == 0, kc == 7)
                    c.op("act", lambda e: e.activation(out=sg[:, dc, :], in_=ps[:], func=AF.Silu), reads=[psB_], writes=[sgB[dc]])
                release(wn)
            for fc in range(16):
                g = fc // 4
                ps, psB_ = pb()
                for tb in range(4):
                    blk = (ti * 4 + tb) % 5
                    prv = (ti * 4 + tb - 1) % 5
                    first_blk = (ti == 0 and tb == 0)
                    o_ap = ps[:, tb * 128:(tb + 1) * 128]
                    bm = lambda m: bandb[:, (3 * g + m) * 128:(3 * g + m + 1) * 128]
                    if first_blk:
                        c.op("pe", lambda e: e.matmul(o_ap, lhsT=vtok[blk][:, fc * 128:(fc + 1) * 128], rhs=bm(2), start=True, stop=True),
                             reads=[vtokB[blk][g], bandB], writes=[psB_], same_ok=True, acc=(tb != 0))
                    else:
                        c.op("pe", lambda e: e.matmul(o_ap, lhsT=vtok[prv][:, fc * 128:(fc + 1) * 128], rhs=bm(1), start=True, stop=False),
                             reads=[vtokB[prv][g], bandB], writes=[psB_], same_ok=True, acc=(tb != 0))
                        c.op("pe", lambda e: e.matmul(o_ap, lhsT=vtok[blk][:, fc * 128:(fc + 1) * 128], rhs=bm(0), start=False, stop=True),
                             reads=[vtokB[blk][g], bandB], writes=[psB_], same_ok=True, acc=True)
                evac_copy(pT[:, fc, :], ps[:], psB_, pTB[fc])
            for g in range(4):
                w, wB, wn = acquire("pgr%d" % g)
                for j in range(4):
                    dc = g * 4 + j
                    ps, psB_ = pb()
                    for kc in range(4):
                        mm(ps[:], psB_, w[:, kc, j * 128:(j + 1) * 128], pT[:, g * 4 + kc, :], [wB, pTB[g * 4 + kc]], kc == 0, kc == 3)
                    c.op("dve", lambda e: e.scalar_tensor_tensor(out=sg[:, dc, :], in0=ps[:], scalar=vt[:, VC_PS + dc:VC_PS + dc + 1], in1=sg[:, dc, :],
                                                                 op0=ALU.mult, op1=ALU.mult),
                         reads=[psB_, vtB, sgB[dc]], writes=[sgB[dc]])
                release(wn)
            out_proj(h, hBl, "p")

            norm_to_hn(h, hBl, VC_G1)
            i_done = [False] * 4
            gate_done = [False] * 4

            def proj_task():
                for g in range(4):
                    w, wB, wn = acquire("hin%d" % (8 + g))
                    for tb in range(4):
                        ps, psB_ = pb()
                        for kc in range(8):
                            mm(ps[:], psB_, hn[:, kc, tb * 128:(tb + 1) * 128], w[:, kc, :], [hnB[kc], wB], kc == 0, kc == 7)
                        evac_copy(vtok2[:, tb, g * 512:(g + 1) * 512], ps[:], psB_, pTB[tb * 4 + g])
                        if tb % 2 == 1:
                            if tb == 3:
                                release(wn)
                                i_done[g] = True
                            yield
                    w, wB, wn = acquire("hin%d" % (12 + g))
                    for j in range(4):
                        dc = g * 4 + j
                        ps, psB_ = pb()
                        for kc in range(8):
                            mm(ps[:], psB_, w[:, kc, j * 128:(j + 1) * 128], hn[:, kc, :], [wB, hnB[kc]], kc == 0, kc == 7)
                        c.op("act", lambda e: e.activation(out=sg[:, dc, :], in_=ps[:], func=AF.Silu), reads=[psB_], writes=[sgB[dc]])
                    release(wn)
                    gate_done[g] = True
                    yield

            quad = {}

            def head_task(hd):
                hq, j4 = divmod(hd, 4)
                while not freeA:
                    yield
                a = freeA.pop(0)
                if j4 == 0:
                    quad[hq] = (acquire("hin%d" % (4 + hq)), acquire("hin%d" % hq))
                (wf, wfB, wfn), (wq, wqB, wqn) = quad[hq]
                r = hd % NH
                fs = slice(j4 * 128, (j4 + 1) * 128)
                ps, psB_ = pb()
                for kc in range(8):
                    mm(ps[:], psB_, wf[:, kc, fs], hn[:, kc, :], [wfB, hnB[kc]], kc == 0, kc == 7)
                c.op("act", lambda e: e.activation(out=R_t[a][:], in_=ps[:], func=AF.Exp), reads=[psB_], writes=[R_B[a]])
                if j4 == 3:
                    release(wfn)
                yield
                c.op("act", lambda e: e.activation(out=L_t[a][:], in_=R_t[a][:], func=AF.Ln, bias=1.0), reads=[R_B[a]], writes=[L_B[a]])
                yield
                c.op("act", lambda e: e.activation(out=R_t[a][:], in_=R_t[a][:], func=AF.Ln, bias=lbc(hd)), reads=[R_B[a], lbB], writes=[R_B[a]])
                yield
                c.op("pool", lambda e: e.tensor_tensor(out=R_t[a][:], in0=R_t[a][:], in1=L_t[a][:], op=ALU.subtract), reads=[R_B[a], L_B[a]], writes=[R_B[a]])
                yield
                c.op("dve", lambda e: e.tensor_tensor_scan(out=B_t[a][:], data0=onesf[:], data1=R_t[a][:], initial=0.0, op0=ALU.mult, op1=ALU.add),
                     reads=[onesfB, R_B[a]], writes=[B_B[a]], n=1024)
                yield
                B3 = B_t[a][:].rearrange("p (j t) -> p j t", t=128)
                c.op("dve", lambda e: e.tensor_tensor(out=R_t[a][:].rearrange("p (j t) -> p j t", t=128), in0=B3,
                                                      in1=B3[:, :, 63:64].broadcast_to([128, 4, 128]), op=ALU.subtract),
                     reads=[B_B[a]], writes=[R_B[a]])
                c.op("pool", lambda e: e.tensor_tensor(out=lc_t[r][:, 1:4], in0=B3[:, 1:4, 63], in1=B3[:, 0:3, 63], op=ALU.subtract),
                     reads=[B_B[a]], writes=[lc_B[r]], n=4)
                c.op("pool", lambda e: e.tensor_tensor(out=lc_t[r][:, 0:1], in0=B3[:, 0, 63:64], in1=carry[:, hd:hd + 1], op=ALU.add),
                     reads=[B_B[a], carry_B[hd]], writes=[lc_B[r]], acc=True, n=4)
                c.op("pool", lambda e: e.tensor_tensor(out=carry[:, hd:hd + 1], in0=B3[:, 3, 127:128], in1=B3[:, 3, 63:64], op=ALU.subtract),
                     reads=[B_B[a]], writes=[carry_B[hd]], n=4)
                yield
                c.op("act", lambda e: e.activation(out=Eq_t[a][:], in_=R_t[a][:], func=AF.Exp), reads=[R_B[a]], writes=[Eq_B[a]])
                c.op("act", lambda e: e.activation(out=cv_t[r][:], in_=lc_t[r][:], func=AF.Exp), reads=[lc_B[r]], writes=[cv_B[r]], n=4)
                c.op("pool", lambda e: e.tensor_tensor(out=L_t[a][:], in0=L_t[a][:], in1=R_t[a][:], op=ALU.add), reads=[R_B[a], L_B[a]], writes=[L_B[a]])
                yield
                c.op("act", lambda e: e.activation(out=kT_t[r][:], in_=L_t[a][:], func=AF.Exp, scale=-1.0, bias=lnomlb(hd)), reads=[L_B[a], lbB], writes=[kT_B[r]])
                yield
                ps, psB_ = pb()
                for kc in range(8):
                    mm(ps[:], psB_, wq[:, kc, fs], hn[:, kc, :], [wqB, hnB[kc]], kc == 0, kc == 7)
                c.op("dve", lambda e: e.tensor_tensor(out=qT_t[r][:], in0=ps[:], in1=Eq_t[a][:], op=ALU.mult), reads=[psB_, Eq_B[a]], writes=[qT_B[r]])
                if j4 == 3:
                    release(wqn)
                freeA.append(a)
                yield
                while not (freeB and i_done[hq]):
                    yield
                b = freeB.pop(0)
                pstv = psm[b][:].bitcast(BF16)
                for j in range(4):
                    c.op("pe", lambda e: e.transpose(out=pstv[:, 512 + j * 128: 512 + (j + 1) * 128], in_=kT_t[r][:, j * 128:(j + 1) * 128], identity=identb[:]),
                         reads=[kT_B[r], identB], writes=[psmB[b]], same_ok=True, acc=(j != 0), n=128)
                c.op("act", lambda e: e.activation(out=ktok_t[r][:], in_=pstv[:, 512:1024], func=AF.Copy), reads=[psmB[b]], writes=[ktok_B[r]])
                yield
                po, poB = pso[b], psoB[b]
                Pv = P_all[:, hd, :]
                for j in range(4):
                    cs = slice(j * 128, (j + 1) * 128)
                    a2 = 2 * r + (j % 2)
                    c.op("pool", lambda e: e.tensor_scalar(out=Sp_t[a2][:], in0=Pv, scalar1=cv_t[r][:, j:j + 1], scalar2=0.0, op0=ALU.mult, op1=ALU.add),
                         reads=[P_B[hd], cv_B[r]], writes=[Sp_B[a2]], n=128)
                    pa = psm[b][:, 0:128]
                    c.op("pe", lambda e: e.matmul(pa, lhsT=kT_t[r][:, cs], rhs=qT_t[r][:, cs], start=True, stop=True),
                         reads=[kT_B[r], qT_B[r]], writes=[psmB[b]], same_ok=True, n=128)
                    c.op("dve", lambda e: e.tensor_tensor(out=At_t[a2][0:64, :], in0=pa[0:64, :], in1=maskf[0:64, :], op=ALU.mult),
                         reads=[psmB[b], maskB], writes=[At_B[a2]], n=128)
                    c.op("dve", lambda e: e.tensor_tensor(out=At_t[a2][64:128, 64:128], in0=pa[64:128, 64:128], in1=maskf[64:128, 64:128], op=ALU.mult),
                         reads=[psmB[b], maskB], writes=[At_B[a2]], acc=True, n=64)
                    yield
                    vsl = vtok2[:, j, hd * 128:(hd + 1) * 128]
                    vB = pTB[j * 4 + hd // 4]
                    c.op("pe", lambda e: e.matmul(po[:, cs], lhsT=vsl, rhs=At_t[a2][:], start=True, stop=False),
                         reads=[vB, At_B[a2]], writes=[poB], same_ok=True, acc=(j != 0), n=128)
                    c.op("pe", lambda e: e.matmul(po[:, cs], lhsT=Sp_t[a2][:], rhs=qT_t[r][:, cs], start=False, stop=True),
                         reads=[Sp_B[a2], qT_B[r]], writes=[poB], same_ok=True, acc=True, n=128)
                    pu = psm[b][:, 128:256]
                    c.op("pe", lambda e: e.matmul(pu, lhsT=ktok_t[r][:, cs], rhs=vsl, start=True, stop=True),
                         reads=[ktok_B[r], vB], writes=[psmB[b]], same_ok=True, n=128)
                    c.op("dve", lambda e: e.scalar_tensor_tensor(out=Pv, in0=Pv, scalar=cv_t[r][:, j:j + 1], in1=pu, op0=ALU.mult, op1=ALU.add),
                         reads=[P_B[hd], cv_B[r], psmB[b]], writes=[P_B[hd]], n=128)
                    yield
                while not gate_done[hq]:
                    yield
                s2 = hd % 2
                s1 = 0
                c.op("act", lambda e: e.activation(out=sq[s2][:], in_=po[:], func=AF.Square), reads=[poB], writes=[sqB[s2]])
                ps, psB_ = pb()
                mm(ps[:], psB_, onesb[:], sq[s2][:], [onesbB, sqB[s2]], True, True)
                c.op("act", lambda e: e.activation(out=rs_t[s1][:], in_=ps[:], func=AF.Ln, scale=1.0 / 128, bias=EPS), reads=[psB_], writes=[rs_B[s1]])
                c.op("act", lambda e: e.activation(out=rs_t[s1][:], in_=rs_t[s1][:], func=AF.Exp, scale=-0.5), reads=[rs_B[s1]], writes=[rs_B[s1]])
                c.op("dve", lambda e: e.scalar_tensor_tensor(out=t1_t[s1][:], in0=po[:], scalar=vt[:, VC_HG + hd:VC_HG + hd + 1], in1=rs_t[s1][:],
                                                             op0=ALU.mult, op1=ALU.mult),
                     reads=[poB, vtB, rs_B[s1]], writes=[t1_B[s1]])
                c.op("pool", lambda e: e.tensor_tensor(out=sg[:, hd, :], in0=t1_t[s1][:], in1=sg[:, hd, :], op=ALU.mult),
                     reads=[t1_B[s1], sgB[hd]], writes=[sgB[hd]], n=850)
                freeB.append(b)

            freeA = list(range(NA))
            freeB = list(range(NB))
            run_tasks(c, [(proj_task(), PROJ_PACE)] + [(head_task(hd), 0.0) for hd in range(16)], NH + 1)
            out_proj(h, hBl, "h")
            rms(h, hBl, None, None, None)
            for k in range(8):
                c.op("dve", lambda e: e.scalar_tensor_tensor(out=h[:, k, :], in0=h[:, k, :], scalar=vt[:, VC_FG + k:VC_FG + k + 1], in1=rstd[:],
                                                             op0=ALU.mult, op1=ALU.mult),
                     reads=[hBl[k], vtB, rstdB], writes=[hBl[k]])
            c.dma("sp", ov[:, :, ti * T:(ti + 1) * T], h[:], reads=hBl)
        c.wait_all("sp", hB[0] + hB[1])
        c.wait_all("act", hB[0] + hB[1])
    if record:
        return nc, order_rec
    return nc, order


_CACHE = {}


def build_program(nt=SEQ // T):
    _, order = build_nc(nt, None)
    nc, _ = build_nc(nt, order)
    return nc


def _host_consts():
    if "c" not in _CACHE:
        s = np.arange(128)[:, None]
        t = np.arange(128)[None, :]
        cm = np.concatenate([np.eye(128, dtype=np.float32), (s <= t).astype(np.float32)], axis=1)
        _CACHE["c"] = (_band_mats(), np.ascontiguousarray(cm))
    return _CACHE["c"]


def make_in_maps(x, norm_g, pool_w_in, pool_w_grp, pool_scale, pool_w_out, hgrn_w_in,
                 hgrn_lower_bounds, hgrn_norm_g, hgrn_w_out, final_g, nt=SEQ // T):
    bands, cm = _host_consts()
    f = lambda a: np.ascontiguousarray(np.asarray(a, np.float32))
    vecs = np.concatenate([_pcol(norm_g[0]), _pcol(norm_g[1]), _pcol(final_g), _pcol(pool_scale[0]),
                           _pcol(hgrn_lower_bounds[0]), _pcol(hgrn_lower_bounds[1]), _pcol(hgrn_norm_g[0])], axis=1)
    assert vecs.shape == (128, NVC)
    shared = {
        "pw_in": f(pool_w_in[0]), "pw_grp": f(np.asarray(pool_w_grp[0]).reshape(2048, 512)), "pw_out": f(pool_w_out[0]),
        "hw_in": f(hgrn_w_in[0]), "hw_out": f(hgrn_w_out[0]), "vecs": f(vecs), "bands": bands, "cmisc": cm,
    }
    x = np.asarray(x, np.float32)
    maps = []
    for b in range(x.shape[0]):
        m = dict(shared)
        m["xT"] = np.ascontiguousarray(x[b, :nt * T, :].T)
        maps.append(m)
    return maps


def kernel(x, norm_g, pool_w_in, pool_w_grp, pool_scale, pool_w_out, hgrn_w_in,
           hgrn_lower_bounds, hgrn_norm_g, hgrn_w_out, final_g):
    maps = make_in_maps(x, norm_g, pool_w_in, pool_w_grp, pool_scale, pool_w_out, hgrn_w_in,
                        hgrn_lower_bounds, hgrn_norm_g, hgrn_w_out, final_g)
    nc = build_program()
    res = run_bass_kernel_spmd(nc, maps, core_ids=list(range(len(maps))))
    out = np.stack([np.ascontiguousarray(r["outT"].T) for r in res.results], axis=0)
    return out.astype(np.float32)
```

```python
import numpy as np
from contextlib import ExitStack
import concourse.bass as bass
import concourse.mybir as mybir
from concourse.bass_utils import run_bass_kernel_spmd

F32 = mybir.dt.float32
BF16 = mybir.dt.bfloat16
ALU = mybir.AluOpType
AF = mybir.ActivationFunctionType

D_MODEL = 1024
SEQ = 4096
D_INNER = 2048
T = 512
EPS = 1e-6
NSLOT = 6
WINDOWS = (2, 4, 8, 16)
HEAD_STAGGER = 3
PROJ_SKIP = 3
NPG = 4


class Buf:
    __slots__ = ("name", "writes", "reads", "dsem", "dcnt")

    def __init__(self, name=""):
        self.name = name
        self.writes = {}
        self.reads = {}
        self.dsem = None
        self.dcnt = 0


class Ctx:
    ENG = ("pe", "act", "dve", "pool", "sp")

    def __init__(self, nc, es):
        self.nc = nc
        self.es = es
        self.e = {"pe": nc.tensor, "act": nc.scalar, "dve": nc.vector, "pool": nc.gpsimd, "sp": nc.sync}
        self.sem = {k: es.enter_context(nc.semaphore("sem_" + k)) for k in self.ENG}
        self.cnt = {k: 0 for k in self.ENG}
        self.seen = {k: {} for k in self.ENG}
        self.nwait = 0
        self.ndsem = 0

    def _deps(self, eng, reads, writes, same_ok=False):
        need = {}
        for b in reads:
            for k, sv in b.writes.items():
                if k not in need or need[k][1] < sv[1]:
                    need[k] = sv
        for b in writes:
            for d in (b.writes, b.reads):
                for k, sv in d.items():
                    if k not in need or need[k][1] < sv[1]:
                        need[k] = sv
        seen = self.seen[eng]
        for k, (s, v) in need.items():
            if k == eng and same_ok:
                continue
            if seen.get(k, 0) >= v:
                continue
            self.e[eng].wait_ge(s, v)
            self.nwait += 1
            seen[k] = v

    def op(self, eng, fn, reads=(), writes=(), same_ok=False, acc=False):
        self._deps(eng, reads, writes, same_ok or acc)
        inst = fn(self.e[eng])
        self.cnt[eng] += 1
        tok = (self.sem[eng], self.cnt[eng])
        inst.then_inc(self.sem[eng], 1)
        for b in reads:
            b.reads[eng] = tok
        for b in writes:
            if acc:
                b.writes[eng] = tok
            else:
                b.writes = {eng: tok}
                b.reads = {}
        return inst

    def dma(self, q, out_ap, in_ap, reads=(), writes=(), **kw):
        self._deps(q, reads, writes)
        owner = writes[0] if writes else reads[0]
        kind = "sw" if q == "pool" else "hw"
        if owner.dsem is None:
            owner.dsem = {}
            owner.dcnt = {}
        if kind not in owner.dsem:
            owner.dsem[kind] = self.es.enter_context(self.nc.semaphore("dsem_%d" % self.ndsem))
            owner.dcnt[kind] = 0
            self.ndsem += 1
        owner.dcnt[kind] += 16
        inst = self.e[q].dma_start(out=out_ap, in_=in_ap, **kw)
        inst.then_inc(owner.dsem[kind], 16)
        key = ("dma", id(owner), kind)
        tok = (owner.dsem[kind], owner.dcnt[kind])
        for b in reads:
            b.reads[key] = tok
        for b in writes:
            b.writes = {key: tok}
            b.reads = {}
        return inst

    def wait_all(self, eng, bufs):
        for b in bufs:
            for d in (b.writes, b.reads):
                for k, (s, v) in d.items():
                    if self.seen[eng].get(k, 0) < v:
                        self.e[eng].wait_ge(s, v)
                        self.seen[eng][k] = v


def run_tasks(gens, k, stagger=0):
    it = iter(gens)
    active = []
    since = stagger
    done = False
    while True:
        if not done and len(active) < k and (since >= stagger or not active):
            g = next(it, None)
            if g is None:
                done = True
            else:
                active.append(g)
                since = 0
        if not active:
            if done:
                break
            continue
        since += 1
        for g in list(active):
            try:
                next(g)
            except StopIteration:
                active.remove(g)


def _band_mats():
    out = np.zeros((128, 12, 128), np.float32)
    s = np.arange(128)[:, None]
    t = np.arange(128)[None, :]
    for g, w in enumerate(WINDOWS):
        d = t - s
        inwin = (d >= 0) & (d < w)
        eye = (d == 0).astype(np.float32)
        out[:, 3 * g + 0, :] = inwin.astype(np.float32) / w - eye
        d2 = t + 128 - s
        out[:, 3 * g + 1, :] = ((d2 >= 0) & (d2 < w)).astype(np.float32) / w
        cnt = np.minimum(t + 1, w).astype(np.float32)
        out[:, 3 * g + 2, :] = inwin.astype(np.float32) / cnt - eye
    return out.reshape(128, 12 * 128)


def _pcol(v):
    v = np.asarray(v, np.float32).reshape(-1, 128)
    return np.ascontiguousarray(v.T)


VC_G0, VC_G1, VC_FG, VC_PS, VC_L0, VC_L1, VC_HG = 0, 8, 16, 24, 40, 56, 72
NVC = 88


def build_nc(nt=SEQ // T, order=None):
    record = order is None
    seq = nt * T
    nc = bass.Bass("TRN2", target_bir_lowering=False)
    xT = nc.dram_tensor("xT", [D_MODEL, seq], F32, kind="ExternalInput").ap()
    outT = nc.dram_tensor("outT", [D_MODEL, seq], F32, kind="ExternalOutput").ap()
    pw_in = nc.dram_tensor("pw_in", [1024, 4096], F32, kind="ExternalInput").ap()
    pw_grp = nc.dram_tensor("pw_grp", [2048, 512], F32, kind="ExternalInput").ap()
    pw_out = nc.dram_tensor("pw_out", [2048, 1024], F32, kind="ExternalInput").ap()
    hw_in = nc.dram_tensor("hw_in", [1024, 8192], F32, kind="ExternalInput").ap()
    hw_out = nc.dram_tensor("hw_out", [2048, 1024], F32, kind="ExternalInput").ap()
    vecs = nc.dram_tensor("vecs", [128, NVC], F32, kind="ExternalInput").ap()
    bands = nc.dram_tensor("bands", [128, 12 * 128], F32, kind="ExternalInput").ap()
    cmisc = nc.dram_tensor("cmisc", [128, 256], F32, kind="ExternalInput").ap()
    NS = 36
    wscr = nc.dram_tensor("wscr", [NS, 128, 4096], BF16, kind="Internal").ap()

    xv = xT.rearrange("(c p) t -> p c t", p=128)
    ov = outT.rearrange("(c p) t -> p c t", p=128)

    with ExitStack() as es:
        c = Ctx(nc, es)
        sb = lambda n, shp, dt=F32: es.enter_context(nc.sbuf_tensor(n, shp, dt))

        hbuf = [sb("hbuf%d" % i, [128, 8, T]) for i in range(2)]
        hB = [[Buf("h%d_%d" % (i, k)) for k in range(8)] for i in range(2)]
        hn = sb("hn", [128, 8, T], BF16)
        hnB = [Buf("hn%d" % k) for k in range(8)]
        sq = [sb("sq%d" % i, [128, T], BF16) for i in range(2)]
        sqB = [Buf("sq%d" % i) for i in range(2)]
        lnt = sb("lnt", [128, T]); lntB = Buf("lnt")
        rstd, rstdB = lnt, lntB
        vtok = [sb("vtok%d" % i, [128, D_INNER], BF16) for i in range(5)]
        vtokB = [[Buf("vtok%d_%d" % (i, g)) for g in range(4)] for i in range(5)]
        pT = sb("pT", [128, 16, T], BF16)
        pTB = [Buf("pT%d" % k) for k in range(16)]
        vtok2 = pT[:].rearrange("p (b g) t -> p b (g t)", b=4)
        sg = sb("sg", [128, 16, T], BF16)
        sgB = [Buf("sg%d" % k) for k in range(16)]
        NA, NH, NB = 3, 5, 2
        R_t = [sb("R_t%d" % i, [128, T]) for i in range(NA)]; R_B = [Buf() for _ in range(NA)]
        L_t = [sb("L_t%d" % i, [128, T]) for i in range(NA)]; L_B = [Buf() for _ in range(NA)]
        B_t = [sb("B_t%d" % i, [128, T]) for i in range(NA)]; B_B = [Buf() for _ in range(NA)]
        Eq_t = [sb("Eq_t%d" % i, [128, T]) for i in range(NA)]; Eq_B = [Buf() for _ in range(NA)]
        kT_t = [sb("kT_t%d" % i, [128, T], BF16) for i in range(NH)]; kT_B = [Buf() for _ in range(NH)]
        qT_t = [sb("qT_t%d" % i, [128, T], BF16) for i in range(NH)]; qT_B = [Buf() for _ in range(NH)]
        ktok_t = [sb("ktok_t%d" % i, [128, T], BF16) for i in range(NH)]; ktok_B = [Buf() for _ in range(NH)]
        At_t = [sb("At_t%d" % i, [128, 128], BF16) for i in range(2 * NH)]; At_B = [Buf() for _ in range(2 * NH)]
        Sp_t = [sb("Sp_t%d" % i, [128, 128], BF16) for i in range(2 * NH)]; Sp_B = [Buf() for _ in range(2 * NH)]
        t1_t = [sb("t1_t%d" % i, [128, T]) for i in range(1)]; t1_B = [Buf() for _ in range(1)]
        rs_t = [sb("rs_t%d" % i, [128, T]) for i in range(1)]; rs_B = [Buf() for _ in range(1)]
        lc_t = [sb("lc_t%d" % i, [128, 4]) for i in range(NH)]; lc_B = [Buf() for _ in range(NH)]
        cv_t = [sb("cv_t%d" % i, [128, 4]) for i in range(NH)]; cv_B = [Buf() for _ in range(NH)]
        P_all = sb("P_all", [128, 16, 128]); P_B = [Buf("P%d" % k) for k in range(16)]
        carry = sb("carry", [128, 16]); carry_B = [Buf("carry%d" % k) for k in range(16)]
        vt = sb("vt", [128, NVC]); vtB = Buf("vt")
        lbt = sb("lbt", [128, 80]); lbB = Buf("lbt")
        bandb = sb("bandb", [128, 12 * 128], BF16); bandB = Buf("band")
        identb = sb("identb", [128, 128], BF16); identB = Buf("ident")
        maskf = sb("maskf", [128, 128]); maskB = Buf("mask")
        onesb = sb("onesb", [128, 128], BF16); onesbB = Buf("onesb")
        onesf = sb("onesf", [128, T]); onesfB = Buf("onesf")
        ring = [sb("ring%d" % i, [128, 8, 512], BF16) for i in range(NSLOT)]
        ringB = [Buf("ring%d" % i) for i in range(NSLOT)]
        scrB = [Buf("scr%d" % i) for i in range(NS)]

        psg = [es.enter_context(nc.psum_tensor("psg%d" % i, [128, 512], F32)) for i in range(NPG)]
        psgB = [Buf("psg%d" % i) for i in range(NPG)]
        pso = [es.enter_context(nc.psum_tensor("pso%d" % i, [128, 512], F32)) for i in range(NB)]
        psoB = [Buf("pso%d" % i) for i in range(NB)]
        psm = [es.enter_context(nc.psum_tensor("psm%d" % i, [128, 512], F32)) for i in range(NB)]
        psmB = [Buf("psm%d" % i) for i in range(NB)]
        st = {"pg": 0, "ev": 0}

        def pb():
            i = st["pg"] % NPG
            st["pg"] += 1
            return psg[i], psgB[i]

        def mm(ps_ap, psB_, lhsT, rhs, rd, first, last):
            c.op("pe", lambda e: e.matmul(ps_ap, lhsT=lhsT, rhs=rhs, start=first, stop=last),
                 reads=rd, writes=[psB_], same_ok=True, acc=not first)

        def evac_copy(out_ap, ps_ap, psB_, outB):
            st["ev"] += 1
            if st["ev"] % 2:
                c.op("act", lambda e: e.activation(out=out_ap, in_=ps_ap, func=AF.Copy), reads=[psB_], writes=[outB])
            else:
                c.op("dve", lambda e: e.tensor_copy(out=out_ap, in_=ps_ap), reads=[psB_], writes=[outB])

        slot_src = []
        skey = {}

        def add_slot(key, src, nk):
            skey[key] = len(slot_src)
            slot_src.append((src, nk))

        pin = pw_in.rearrange("(kc p) f -> p kc f", p=128)
        for g in range(8):
            add_slot("pin%d" % g, pin[:, :, g * 512:(g + 1) * 512], 8)
        pgr = pw_grp.rearrange("(r p) f -> p r f", p=128)
        for g in range(4):
            add_slot("pgr%d" % g, pgr[:, g * 4:(g + 1) * 4, :], 4)
        pou = pw_out.rearrange("(kc p) f -> p kc f", p=128)
        for cg in range(2):
            for half in range(2):
                add_slot("pou%d%d" % (cg, half), pou[:, half * 8:(half + 1) * 8, cg * 512:(cg + 1) * 512], 8)
        hin = hw_in.rearrange("(kc p) f -> p kc f", p=128)
        for g in range(16):
            add_slot("hin%d" % g, hin[:, :, g * 512:(g + 1) * 512], 8)
        hou = hw_out.rearrange("(kc p) f -> p kc f", p=128)
        for cg in range(2):
            for half in range(2):
                add_slot("hou%d%d" % (cg, half), hou[:, half * 8:(half + 1) * 8, cg * 512:(cg + 1) * 512], 8)
        assert len(slot_src) == NS

        c.op("pool", lambda e: e.memset(onesf[:], 1.0), writes=[onesfB])
        c.op("pool", lambda e: e.memset(onesb[:], 1.0), writes=[onesbB])
        c.op("pool", lambda e: e.memset(P_all[:], 0.0), writes=P_B)
        c.op("pool", lambda e: e.memset(carry[:], 0.0), writes=carry_B)
        for i in range(2 * NH):
            c.op("pool", lambda e: e.memset(At_t[i][:], 0.0), writes=[At_B[i]])
        c.dma("sp", vt[:], vecs, writes=[vtB])
        c.dma("sp", maskf[:], cmisc[:, 128:256], writes=[maskB])
        c.dma("pool", identb[:], cmisc[:, 0:128], writes=[identB])
        c.dma("pool", bandb[:], bands, writes=[bandB])

        rs_ = {"emitted": 0, "acq": 0}
        free_slots = list(range(NSLOT))
        load_slot = {}
        order_rec = []
        total_loads = nt * NS

        def emit_load(n, key):
            s_ = skey[key]
            i = free_slots.pop(0)
            load_slot[n] = i
            src, nk = slot_src[s_]
            scr_v = wscr[s_].rearrange("p (kc f) -> p kc f", f=512)[:, 0:nk, :]
            if n < NS:
                c.dma("pool", ring[i][:, 0:nk, :], src, writes=[ringB[i]])
                c.dma("sp", scr_v, ring[i][:, 0:nk, :], reads=[ringB[i]], writes=[scrB[s_]])
            else:
                c.dma("sp", ring[i][:, 0:nk, :], scr_v, reads=[scrB[s_]], writes=[ringB[i]])
            rs_["emitted"] += 1

        def ring_pump():
            if record:
                return
            while rs_["emitted"] < total_loads and free_slots:
                if rs_["acq"] < 3 and rs_["emitted"] - rs_["acq"] >= 2:
                    break
                m = rs_["emitted"]
                emit_load(m, order[m % len(order)])

        def acquire(key):
            n = rs_["acq"]
            rs_["acq"] += 1
            if record:
                order_rec.append(key)
                assert free_slots, "too many weight slots held at once"
                emit_load(n, key)
            else:
                assert order[n % len(order)] == key, (n, key, order[n % len(order)])
                assert n < rs_["emitted"], "ring underflow"
            i = load_slot[n]
            ring_pump()
            return ring[i], ringB[i], n

        def release(*ns):
            for n in ns:
                free_slots.append(load_slot.pop(n))
            ring_pump()

        c.op("dve", lambda e: e.tensor_tensor(out=lbt[:, 0:16], in0=vt[:, VC_L1:VC_L1 + 16], in1=vt[:, VC_L0:VC_L0 + 16], op=ALU.subtract),
             reads=[vtB], writes=[lbB])
        c.op("act", lambda e: e.activation(out=lbt[:, 0:16], in_=lbt[:, 0:16], func=AF.Exp), reads=[lbB], writes=[lbB])
        c.op("dve", lambda e: e.tensor_scalar(out=lbt[:, 0:16], in0=lbt[:, 0:16], scalar1=1.0, scalar2=None, op0=ALU.add), reads=[lbB], writes=[lbB])
        c.op("dve", lambda e: e.reciprocal(out=lbt[:, 16:32], in_=lbt[:, 0:16]), reads=[lbB], writes=[lbB])
        c.op("dve", lambda e: e.tensor_scalar(out=lbt[:, 32:48], in0=lbt[:, 16:32], scalar1=-1.0, scalar2=None, op0=ALU.mult), reads=[lbB], writes=[lbB])
        c.op("dve", lambda e: e.tensor_scalar(out=lbt[:, 48:64], in0=lbt[:, 32:48], scalar1=1.0, scalar2=None, op0=ALU.add), reads=[lbB], writes=[lbB])
        c.op("act", lambda e: e.activation(out=lbt[:, 64:80], in_=lbt[:, 16:32], func=AF.Ln), reads=[lbB], writes=[lbB])
        omlb = lambda hd: lbt[:, 16 + hd:17 + hd]
        lbc = lambda hd: lbt[:, 48 + hd:49 + hd]
        lnomlb = lambda hd: lbt[:, 64 + hd:65 + hd]
        nomlb = lambda hd: lbt[:, 32 + hd:33 + hd]

        ring_pump()

        def load_x(ti):
            i = ti % 2
            c.dma("sp", hbuf[i][:], xv[:, :, ti * T:(ti + 1) * T], writes=hB[i])

        def rms(h, hBl, inv_n_chunks, gcol, out_fn):
            ps, psB_ = pb()
            for k in range(8):
                j = k % 2
                c.op("act", lambda e: e.activation(out=sq[j][:], in_=h[:, k, :], func=AF.Square), reads=[hBl[k]], writes=[sqB[j]])
                mm(ps[:], psB_, onesb[:], sq[j][:], [onesbB, sqB[j]], k == 0, k == 7)
            c.op("act", lambda e: e.activation(out=lnt[:], in_=ps[:], func=AF.Ln, scale=1.0 / D_MODEL, bias=EPS), reads=[psB_], writes=[lntB])
            c.op("act", lambda e: e.activation(out=rstd[:], in_=lnt[:], func=AF.Exp, scale=-0.5), reads=[lntB], writes=[rstdB])

        def norm_to_hn(h, hBl, gc):
            rms(h, hBl, None, None, None)
            for k in range(8):
                c.op("dve", lambda e: e.scalar_tensor_tensor(out=hn[:, k, :], in0=h[:, k, :], scalar=vt[:, gc + k:gc + k + 1], in1=rstd[:],
                                                             op0=ALU.mult, op1=ALU.mult),
                     reads=[hBl[k], vtB, rstdB], writes=[hnB[k]])

        def out_proj(h, hBl, lay):
            for cg in range(2):
                w0, w0B, n0 = acquire("%sou%d0" % (lay, cg))
                w1, w1B, n1 = acquire("%sou%d1" % (lay, cg))
                for j in range(4):
                    oc = cg * 4 + j
                    ps, psB_ = pb()
                    for half, (w, wB) in enumerate(((w0, w0B), (w1, w1B))):
                        for kc in range(8):
                            k = half * 8 + kc
                            mm(ps[:], psB_, w[:, kc, j * 128:(j + 1) * 128], sg[:, k, :], [wB, sgB[k]], k == 0, k == 15)
                    c.op("dve", lambda e: e.tensor_tensor(out=h[:, oc, :], in0=ps[:], in1=h[:, oc, :], op=ALU.add),
                         reads=[psB_, hBl[oc]], writes=[hBl[oc]])
                release(n0, n1)

        load_x(0)
        for ti in range(nt):
            hi = ti % 2
            h, hBl = hbuf[hi], hB[hi]
            norm_to_hn(h, hBl, VC_G0)
            for g in range(4):
                w, wB, wn = acquire("pin%d" % g)
                for tb in range(4):
                    blk = (ti * 4 + tb) % 5
                    ps, psB_ = pb()
                    for kc in range(8):
                        mm(ps[:], psB_, hn[:, kc, tb * 128:(tb + 1) * 128], w[:, kc, :], [hnB[kc], wB], kc == 0, kc == 7)
                    evac_copy(vtok[blk][:, g * 512:(g + 1) * 512], ps[:], psB_, vtokB[blk][g])
                release(wn)
            for g in range(4):
                w, wB, wn = acquire("pin%d" % (4 + g))
                for j in range(4):
                    dc = g * 4 + j
                    ps, psB_ = pb()
                    for kc in range(8):
                        mm(ps[:], psB_, w[:, kc, j * 128:(j + 1) * 128], hn[:, kc, :], [wB, hnB[kc]], kc == 0, kc == 7)
                    c.op("act", lambda e: e.activation(out=sg[:, dc, :], in_=ps[:], func=AF.Silu), reads=[psB_], writes=[sgB[dc]])
                release(wn)
            for fc in range(16):
                g = fc // 4
                ps, psB_ = pb()
                for tb in range(4):
                    blk = (ti * 4 + tb) % 5
                    prv = (ti * 4 + tb - 1) % 5
                    first_blk = (ti == 0 and tb == 0)
                    o_ap = ps[:, tb * 128:(tb + 1) * 128]
                    bm = lambda m: bandb[:, (3 * g + m) * 128:(3 * g + m + 1) * 128]
                    if first_blk:
                        c.op("pe", lambda e: e.matmul(o_ap, lhsT=vtok[blk][:, fc * 128:(fc + 1) * 128], rhs=bm(2), start=True, stop=True),
                             reads=[vtokB[blk][g], bandB], writes=[psB_], same_ok=True, acc=(tb != 0))
                    else:
                        c.op("pe", lambda e: e.matmul(o_ap, lhsT=vtok[prv][:, fc * 128:(fc + 1) * 128], rhs=bm(1), start=True, stop=False),
                             reads=[vtokB[prv][g], bandB], writes=[psB_], same_ok=True, acc=(tb != 0))
                        c.op("pe", lambda e: e.matmul(o_ap, lhsT=vtok[blk][:, fc * 128:(fc + 1) * 128], rhs=bm(0), start=False, stop=True),
                             reads=[vtokB[blk][g], bandB], writes=[psB_], same_ok=True, acc=True)
                evac_copy(pT[:, fc, :], ps[:], psB_, pTB[fc])
            for g in range(4):
                w, wB, wn = acquire("pgr%d" % g)
                for j in range(4):
                    dc = g * 4 + j
                    ps, psB_ = pb()
                    for kc in range(4):
                        mm(ps[:], psB_, w[:, kc, j * 128:(j + 1) * 128], pT[:, g * 4 + kc, :], [wB, pTB[g * 4 + kc]], kc == 0, kc == 3)
                    c.op("dve", lambda e: e.scalar_tensor_tensor(out=sg[:, dc, :], in0=ps[:], scalar=vt[:, VC_PS + dc:VC_PS + dc + 1], in1=sg[:, dc, :],
                                                                 op0=ALU.mult, op1=ALU.mult),
                         reads=[psB_, vtB, sgB[dc]], writes=[sgB[dc]])
                release(wn)
            out_proj(h, hBl, "p")
            if ti + 1 < nt:
                load_x(ti + 1)

            norm_to_hn(h, hBl, VC_G1)
            i_done = [False] * 4
            gate_done = [False] * 4

            def proj_task():
                for g in range(4):
                    w, wB, wn = acquire("hin%d" % (8 + g))
                    for tb in range(4):
                        ps, psB_ = pb()
                        for kc in range(8):
                            mm(ps[:], psB_, hn[:, kc, tb * 128:(tb + 1) * 128], w[:, kc, :], [hnB[kc], wB], kc == 0, kc == 7)
                        evac_copy(vtok2[:, tb, g * 512:(g + 1) * 512], ps[:], psB_, pTB[tb * 4 + g])
                        if tb % 2 == 1:
                            if tb == 3:
                                release(wn)
                                i_done[g] = True
                            for _ in range(PROJ_SKIP + 1):
                                yield
                    w, wB, wn = acquire("hin%d" % (12 + g))
                    for j in range(4):
                        dc = g * 4 + j
                        ps, psB_ = pb()
                        for kc in range(8):
                            mm(ps[:], psB_, w[:, kc, j * 128:(j + 1) * 128], hn[:, kc, :], [wB, hnB[kc]], kc == 0, kc == 7)
                        c.op("act", lambda e: e.activation(out=sg[:, dc, :], in_=ps[:], func=AF.Silu), reads=[psB_], writes=[sgB[dc]])
                    release(wn)
                    gate_done[g] = True
                    for _ in range(PROJ_SKIP + 1):
                        yield

            quad = {}

            def head_task(hd):
                hq, j4 = divmod(hd, 4)
                while not freeA:
                    yield
                a = freeA.pop(0)
                if j4 == 0:
                    quad[hq] = (acquire("hin%d" % (4 + hq)), acquire("hin%d" % hq))
                (wf, wfB, wfn), (wq, wqB, wqn) = quad[hq]
                r = hd % NH
                fs = slice(j4 * 128, (j4 + 1) * 128)
                ps, psB_ = pb()
                for kc in range(8):
                    mm(ps[:], psB_, wf[:, kc, fs], hn[:, kc, :], [wfB, hnB[kc]], kc == 0, kc == 7)
                c.op("act", lambda e: e.activation(out=R_t[a][:], in_=ps[:], func=AF.Exp), reads=[psB_], writes=[R_B[a]])
                if j4 == 3:
                    release(wfn)
                yield
                c.op("act", lambda e: e.activation(out=L_t[a][:], in_=R_t[a][:], func=AF.Ln, bias=1.0), reads=[R_B[a]], writes=[L_B[a]])
                yield
                c.op("act", lambda e: e.activation(out=R_t[a][:], in_=R_t[a][:], func=AF.Ln, bias=lbc(hd)), reads=[R_B[a], lbB], writes=[R_B[a]])
                yield
                c.op("pool", lambda e: e.tensor_tensor(out=R_t[a][:], in0=R_t[a][:], in1=L_t[a][:], op=ALU.subtract), reads=[R_B[a], L_B[a]], writes=[R_B[a]])
                yield
                c.op("dve", lambda e: e.tensor_tensor_scan(out=B_t[a][:], data0=onesf[:], data1=R_t[a][:], initial=0.0, op0=ALU.mult, op1=ALU.add),
                     reads=[onesfB, R_B[a]], writes=[B_B[a]])
                yield
                B3 = B_t[a][:].rearrange("p (j t) -> p j t", t=128)
                c.op("dve", lambda e: e.tensor_tensor(out=R_t[a][:].rearrange("p (j t) -> p j t", t=128), in0=B3,
                                                      in1=B3[:, :, 63:64].broadcast_to([128, 4, 128]), op=ALU.subtract),
                     reads=[B_B[a]], writes=[R_B[a]])
                c.op("pool", lambda e: e.tensor_tensor(out=lc_t[r][:, 1:4], in0=B3[:, 1:4, 63], in1=B3[:, 0:3, 63], op=ALU.subtract),
                     reads=[B_B[a]], writes=[lc_B[r]])
                c.op("pool", lambda e: e.tensor_tensor(out=lc_t[r][:, 0:1], in0=B3[:, 0, 63:64], in1=carry[:, hd:hd + 1], op=ALU.add),
                     reads=[B_B[a], carry_B[hd]], writes=[lc_B[r]], acc=True)
                c.op("pool", lambda e: e.tensor_tensor(out=carry[:, hd:hd + 1], in0=B3[:, 3, 127:128], in1=B3[:, 3, 63:64], op=ALU.subtract),
                     reads=[B_B[a]], writes=[carry_B[hd]])
                yield
                c.op("act", lambda e: e.activation(out=Eq_t[a][:], in_=R_t[a][:], func=AF.Exp), reads=[R_B[a]], writes=[Eq_B[a]])
                c.op("act", lambda e: e.activation(out=cv_t[r][:], in_=lc_t[r][:], func=AF.Exp), reads=[lc_B[r]], writes=[cv_B[r]])
                c.op("pool", lambda e: e.tensor_tensor(out=L_t[a][:], in0=L_t[a][:], in1=R_t[a][:], op=ALU.add), reads=[R_B[a], L_B[a]], writes=[L_B[a]])
                yield
                c.op("act", lambda e: e.activation(out=kT_t[r][:], in_=L_t[a][:], func=AF.Exp, scale=-1.0, bias=lnomlb(hd)), reads=[L_B[a], lbB], writes=[kT_B[r]])
                yield
                ps, psB_ = pb()
                for kc in range(8):
                    mm(ps[:], psB_, wq[:, kc, fs], hn[:, kc, :], [wqB, hnB[kc]], kc == 0, kc == 7)
                c.op("dve", lambda e: e.tensor_tensor(out=qT_t[r][:], in0=ps[:], in1=Eq_t[a][:], op=ALU.mult), reads=[psB_, Eq_B[a]], writes=[qT_B[r]])
                if j4 == 3:
                    release(wqn)
                freeA.append(a)
                yield
                while not (freeB and i_done[hq]):
                    yield
                b = freeB.pop(0)
                pstv = psm[b][:].bitcast(BF16)
                for j in range(4):
                    c.op("pe", lambda e: e.transpose(out=pstv[:, 512 + j * 128: 512 + (j + 1) * 128], in_=kT_t[r][:, j * 128:(j + 1) * 128], identity=identb[:]),
                         reads=[kT_B[r], identB], writes=[psmB[b]], same_ok=True, acc=(j != 0))
                c.op("act", lambda e: e.activation(out=ktok_t[r][:], in_=pstv[:, 512:1024], func=AF.Copy), reads=[psmB[b]], writes=[ktok_B[r]])
                yield
                po, poB = pso[b], psoB[b]
                Pv = P_all[:, hd, :]
                for j in range(4):
                    cs = slice(j * 128, (j + 1) * 128)
                    a2 = 2 * r + (j % 2)
                    c.op("pool", lambda e: e.tensor_scalar(out=Sp_t[a2][:], in0=Pv, scalar1=cv_t[r][:, j:j + 1], scalar2=0.0, op0=ALU.mult, op1=ALU.add),
                         reads=[P_B[hd], cv_B[r]], writes=[Sp_B[a2]])
                    pa = psm[b][:, 0:128]
                    c.op("pe", lambda e: e.matmul(pa, lhsT=kT_t[r][:, cs], rhs=qT_t[r][:, cs], start=True, stop=True),
                         reads=[kT_B[r], qT_B[r]], writes=[psmB[b]], same_ok=True)
                    c.op("dve", lambda e: e.tensor_tensor(out=At_t[a2][0:64, :], in0=pa[0:64, :], in1=maskf[0:64, :], op=ALU.mult),
                         reads=[psmB[b], maskB], writes=[At_B[a2]])
                    c.op("dve", lambda e: e.tensor_tensor(out=At_t[a2][64:128, 64:128], in0=pa[64:128, 64:128], in1=maskf[64:128, 64:128], op=ALU.mult),
                         reads=[psmB[b], maskB], writes=[At_B[a2]], acc=True)
                    yield
                    vsl = vtok2[:, j, hd * 128:(hd + 1) * 128]
                    vB = pTB[j * 4 + hd // 4]
                    c.op("pe", lambda e: e.matmul(po[:, cs], lhsT=vsl, rhs=At_t[a2][:], start=True, stop=False),
                         reads=[vB, At_B[a2]], writes=[poB], same_ok=True, acc=(j != 0))
                    c.op("pe", lambda e: e.matmul(po[:, cs], lhsT=Sp_t[a2][:], rhs=qT_t[r][:, cs], start=False, stop=True),
                         reads=[Sp_B[a2], qT_B[r]], writes=[poB], same_ok=True, acc=True)
                    pu = psm[b][:, 128:256]
                    c.op("pe", lambda e: e.matmul(pu, lhsT=ktok_t[r][:, cs], rhs=vsl, start=True, stop=True),
                         reads=[ktok_B[r], vB], writes=[psmB[b]], same_ok=True)
                    c.op("dve", lambda e: e.scalar_tensor_tensor(out=Pv, in0=Pv, scalar=cv_t[r][:, j:j + 1], in1=pu, op0=ALU.mult, op1=ALU.add),
                         reads=[P_B[hd], cv_B[r], psmB[b]], writes=[P_B[hd]])
                    yield
                while not gate_done[hq]:
                    yield
                s2 = hd % 2
                s1 = 0
                c.op("act", lambda e: e.activation(out=sq[s2][:], in_=po[:], func=AF.Square), reads=[poB], writes=[sqB[s2]])
                ps, psB_ = pb()
                mm(ps[:], psB_, onesb[:], sq[s2][:], [onesbB, sqB[s2]], True, True)
                c.op("act", lambda e: e.activation(out=rs_t[s1][:], in_=ps[:], func=AF.Ln, scale=1.0 / 128, bias=EPS), reads=[psB_], writes=[rs_B[s1]])
                c.op("act", lambda e: e.activation(out=rs_t[s1][:], in_=rs_t[s1][:], func=AF.Exp, scale=-0.5), reads=[rs_B[s1]], writes=[rs_B[s1]])
                c.op("dve", lambda e: e.scalar_tensor_tensor(out=t1_t[s1][:], in0=po[:], scalar=vt[:, VC_HG + hd:VC_HG + hd + 1], in1=rs_t[s1][:],
                                                             op0=ALU.mult, op1=ALU.mult),
                     reads=[poB, vtB, rs_B[s1]], writes=[t1_B[s1]])
                c.op("pool", lambda e: e.tensor_tensor(out=sg[:, hd, :], in0=t1_t[s1][:], in1=sg[:, hd, :], op=ALU.mult),
                     reads=[t1_B[s1], sgB[hd]], writes=[sgB[hd]])
                freeB.append(b)

            freeA = list(range(NA))
            freeB = list(range(NB))
            run_tasks([proj_task()] + [head_task(hd) for hd in range(16)], NH + 1, stagger=HEAD_STAGGER)
            out_proj(h, hBl, "h")
            rms(h, hBl, None, None, None)
            for k in range(8):
                c.op("dve", lambda e: e.scalar_tensor_tensor(out=h[:, k, :], in0=h[:, k, :], scalar=vt[:, VC_FG + k:VC_FG + k + 1], in1=rstd[:],
                                                             op0=ALU.mult, op1=ALU.mult),
                     reads=[hBl[k], vtB, rstdB], writes=[hBl[k]])
            c.dma("sp", ov[:, :, ti * T:(ti + 1) * T], h[:], reads=hBl)
        c.wait_all("sp", hB[0] + hB[1])
        c.wait_all("act", hB[0] + hB[1])
    if record:
        return nc, order_rec
    return nc, order


_CACHE = {}


def build_program(nt=SEQ // T):
    _, order = build_nc(1, None)
    nc, _ = build_nc(nt, order)
    return nc


def _host_consts():
    if "c" not in _CACHE:
        s = np.arange(128)[:, None]
        t = np.arange(128)[None, :]
        cm = np.concatenate([np.eye(128, dtype=np.float32), (s <= t).astype(np.float32)], axis=1)
        _CACHE["c"] = (_band_mats(), np.ascontiguousarray(cm))
    return _CACHE["c"]


def make_in_maps(x, norm_g, pool_w_in, pool_w_grp, pool_scale, pool_w_out, hgrn_w_in,
                 hgrn_lower_bounds, hgrn_norm_g, hgrn_w_out, final_g, nt=SEQ // T):
    bands, cm = _host_consts()
    f = lambda a: np.ascontiguousarray(np.asarray(a, np.float32))
    vecs = np.concatenate([_pcol(norm_g[0]), _pcol(norm_g[1]), _pcol(final_g), _pcol(pool_scale[0]),
                           _pcol(hgrn_lower_bounds[0]), _pcol(hgrn_lower_bounds[1]), _pcol(hgrn_norm_g[0])], axis=1)
    assert vecs.shape == (128, NVC)
    shared = {
        "pw_in": f(pool_w_in[0]), "pw_grp": f(np.asarray(pool_w_grp[0]).reshape(2048, 512)), "pw_out": f(pool_w_out[0]),
        "hw_in": f(hgrn_w_in[0]), "hw_out": f(hgrn_w_out[0]), "vecs": f(vecs), "bands": bands, "cmisc": cm,
    }
    x = np.asarray(x, np.float32)
    maps = []
    for b in range(x.shape[0]):
        m = dict(shared)
        m["xT"] = np.ascontiguousarray(x[b, :nt * T, :].T)
        maps.append(m)
    return maps


def kernel(x, norm_g, pool_w_in, pool_w_grp, pool_scale, pool_w_out, hgrn_w_in,
           hgrn_lower_bounds, hgrn_norm_g, hgrn_w_out, final_g):
    maps = make_in_maps(x, norm_g, pool_w_in, pool_w_grp, pool_scale, pool_w_out, hgrn_w_in,
                        hgrn_lower_bounds, hgrn_norm_g, hgrn_w_out, final_g)
    nc = build_program()
    res = run_bass_kernel_spmd(nc, maps, core_ids=list(range(len(maps))))
    out = np.stack([np.ascontiguousarray(r["outT"].T) for r in res.results], axis=0)
    return out.astype(np.float32)
```
